# Optimizing a Trainium2 kernel written in Bass

```python
import math
import jax, jax.numpy as jnp
from jax import lax
import numpy as np

D_MODEL = 2048
BATCH = 4
SEQ = 4096
DEPTH = 2

HEAD_DIM = 128
N_HEADS_BRANCH = 4
BRANCH_WIDTH = N_HEADS_BRANCH * HEAD_DIM
N_BRANCHES = 4
MIX_WIDTH = N_BRANCHES * BRANCH_WIDTH
MLA_Q_RANK = 384
MLA_KV_RANK = 256
MLA_NOPE = 128
MLA_ROPE = 64
MLA_V = 128
ROPE_THETA = 10000.0
DILATED_PATTERNS = ((128, 1), (512, 4), (2048, 16))
IDX_HEADS = 16
IDX_DIM = 64
TOPK_MAX = 256
DIFF_QK_DIM = HEAD_DIM // 2
REL_BUCKETS = 32
REL_MAX_DIST = 2048
N_BIAS_HEADS = 3 * N_HEADS_BRANCH
Q_BLOCK = 128
NORM_EPS = 1e-6
NEG_INF = -1e30

IN_SPLITS = (
    ("a_cq", MLA_Q_RANK), ("a_ckv", MLA_KV_RANK), ("a_krope", MLA_ROPE),
    ("b_q", BRANCH_WIDTH), ("b_k", BRANCH_WIDTH), ("b_v", BRANCH_WIDTH),
    ("c_q", BRANCH_WIDTH), ("c_k", BRANCH_WIDTH), ("c_v", BRANCH_WIDTH),
    ("c_qidx", IDX_HEADS * IDX_DIM), ("c_kidx", IDX_DIM), ("c_widx", IDX_HEADS),
    ("d_q", BRANCH_WIDTH), ("d_k", BRANCH_WIDTH), ("d_v", BRANCH_WIDTH),
    ("gate", MIX_WIDTH),
)
IN_WIDTH = 3 * MLA_ROPE + MLA_Q_RANK + MLA_KV_RANK - 2 * MLA_ROPE + 9 * BRANCH_WIDTH + IDX_HEADS * IDX_DIM + IDX_DIM + IDX_HEADS + MIX_WIDTH

kernel_name = "hybrid_mla_dilated_dsa_diff_trunk"


def rmsnorm(x, g):
    xf = x.astype(jnp.float32)
    y = xf * lax.rsqrt(jnp.mean(xf * xf, axis=-1, keepdims=True) + NORM_EPS)
    return (y * g.astype(jnp.float32)).astype(x.dtype)


def split_columns(proj):
    parts, start = {}, 0
    for name, width in IN_SPLITS:
        parts[name] = proj[..., start:start + width]
        start += width
    return parts


def t5_bucket(n):
    max_exact = REL_BUCKETS // 2
    nf = jnp.maximum(n, max_exact).astype(jnp.float32)
    large = max_exact + (jnp.log(nf / max_exact) / math.log(REL_MAX_DIST / max_exact)
                         * (REL_BUCKETS - max_exact)).astype(jnp.int32)
    large = jnp.minimum(large, REL_BUCKETS - 1)
    return jnp.where(n < max_exact, n, large)


def apply_rope(x, pos):
    half = x.shape[-1] // 2
    inv = ROPE_THETA ** (-jnp.arange(half, dtype=jnp.float32) / half)
    ang = pos.astype(jnp.float32)[:, None] * inv[None, :]
    cos = jnp.cos(ang)[None, :, None, :].astype(x.dtype)
    sin = jnp.sin(ang)[None, :, None, :].astype(x.dtype)
    x1, x2 = x[..., :half], x[..., half:]
    return jnp.concatenate([x1 * cos - x2 * sin, x2 * cos + x1 * sin], axis=-1)


def to_blocks(t):
    b, s = t.shape[:2]
    return jnp.swapaxes(t.reshape((b, s // Q_BLOCK, Q_BLOCK) + t.shape[2:]), 0, 1)


def from_blocks(t):
    t = jnp.swapaxes(t, 0, 1)
    return t.reshape((t.shape[0], t.shape[1] * t.shape[2]) + t.shape[3:])


def causal_block_attention(q, k, v, scale):
    s = q.shape[1]
    kpos = jnp.arange(s)

    def one_block(args):
        q_blk, bi = args
        qpos = bi * Q_BLOCK + jnp.arange(Q_BLOCK)
        logits = jnp.einsum('bqhd,bshd->bqhs', q_blk, k).astype(jnp.float32) * scale
        logits = jnp.where((kpos[None, :] <= qpos[:, None])[None, :, None, :], logits, NEG_INF)
        p = jax.nn.softmax(logits, axis=-1).astype(v.dtype)
        return jnp.einsum('bqhs,bshd->bqhd', p, v)

    out = lax.map(one_block, (to_blocks(q), jnp.arange(s // Q_BLOCK)))
    return from_blocks(out)


def dilated_mixture_attention(q, k, v, bias_tab):
    s, dh = q.shape[1], q.shape[-1]
    scale = dh ** -0.5
    patterns = []
    for window, dil in DILATED_PATTERNS:
        offs = dil * jnp.arange(window // dil + 1)
        bias = bias_tab[t5_bucket(offs)].T
        patterns.append((offs, bias))

    def one_block(args):
        q_blk, bi = args
        qpos = bi * Q_BLOCK + jnp.arange(Q_BLOCK)
        lses, outs = [], []
        for offs, bias in patterns:
            kidx = qpos[:, None] - offs[None, :]
            valid = kidx >= 0
            kidx = jnp.maximum(kidx, 0)
            k_g = k[:, kidx]
            v_g = v[:, kidx]
            logits = jnp.einsum('bqhd,bqnhd->bqhn', q_blk, k_g).astype(jnp.float32) * scale
            logits = logits + bias.astype(jnp.float32)[None, None]
            logits = jnp.where(valid[None, :, None, :], logits, NEG_INF)
            lse = jax.nn.logsumexp(logits, axis=-1)
            p = jnp.exp(logits - lse[..., None]).astype(v.dtype)
            outs.append(jnp.einsum('bqhn,bqnhd->bqhd', p, v_g))
            lses.append(lse)
        wts = jax.nn.softmax(jnp.stack(lses, axis=-1), axis=-1).astype(v.dtype)
        return sum(wts[..., g, None] * outs[g] for g in range(len(outs)))

    out = lax.map(one_block, (to_blocks(q), jnp.arange(s // Q_BLOCK)))
    return from_blocks(out)


def dsa_attention(q, k, v, q_idx, k_idx, w_idx, bias_tab):
    s, dh = q.shape[1], q.shape[-1]
    scale = dh ** -0.5
    n_sel = min(TOPK_MAX, s // 4)
    kpos = jnp.arange(s)

    def one_block(args):
        q_blk, qi_blk, wi_blk, bi = args
        qpos = bi * Q_BLOCK + jnp.arange(Q_BLOCK)
        causal = kpos[None, :] <= qpos[:, None]
        dots = jnp.einsum('bqhd,bsd->bqhs', qi_blk, k_idx).astype(jnp.float32) * IDX_DIM ** -0.5
        score = jnp.einsum('bqhs,bqh->bqs', jax.nn.relu(dots),
                           wi_blk.astype(jnp.float32) * IDX_HEADS ** -0.5)
        score = jnp.where(causal[None], score, NEG_INF)
        _, sel = lax.top_k(score, n_sel)
        valid = sel <= qpos[None, :, None]
        k_sel = jax.vmap(lambda kb, ib: kb[ib])(k, sel)
        v_sel = jax.vmap(lambda vb, ib: vb[ib])(v, sel)
        bias = bias_tab[t5_bucket(jnp.maximum(qpos[None, :, None] - sel, 0))]
        logits = jnp.einsum('bqhd,bqkhd->bqhk', q_blk, k_sel).astype(jnp.float32) * scale
        logits = logits + jnp.transpose(bias, (0, 1, 3, 2)).astype(jnp.float32)
        logits = jnp.where(valid[:, :, None, :], logits, NEG_INF)
        p = jax.nn.softmax(logits, axis=-1).astype(v.dtype)
        return jnp.einsum('bqhk,bqkhd->bqhd', p, v_sel)

    out = lax.map(one_block, (to_blocks(q), to_blocks(q_idx), to_blocks(w_idx),
                              jnp.arange(s // Q_BLOCK)))
    return from_blocks(out)


def diff_attention(q1, q2, k1, k2, v, lam, bias_tab):
    s, dq = q1.shape[1], q1.shape[-1]
    scale = dq ** -0.5
    kpos = jnp.arange(s)

    def one_block(args):
        q1b, q2b, bi = args
        qpos = bi * Q_BLOCK + jnp.arange(Q_BLOCK)
        causal = (kpos[None, :] <= qpos[:, None])[None, :, None, :]
        rel = jnp.maximum(qpos[:, None] - kpos[None, :], 0)
        bias = jnp.transpose(bias_tab[t5_bucket(rel)], (0, 2, 1))[None].astype(jnp.float32)
        l1 = jnp.einsum('bqhd,bshd->bqhs', q1b, k1).astype(jnp.float32) * scale + bias
        l2 = jnp.einsum('bqhd,bshd->bqhs', q2b, k2).astype(jnp.float32) * scale + bias
        p1 = jax.nn.softmax(jnp.where(causal, l1, NEG_INF), axis=-1)
        p2 = jax.nn.softmax(jnp.where(causal, l2, NEG_INF), axis=-1)
        a = (p1 - lam * p2).astype(v.dtype)
        return jnp.einsum('bqhs,bshd->bqhd', a, v)

    out = lax.map(one_block, (to_blocks(q1), to_blocks(q2), jnp.arange(s // Q_BLOCK)))
    return from_blocks(out)


def setup_inputs(seed: int = 0) -> dict:
    key = jax.random.key(seed)
    ks = jax.random.split(key, 20)
    f32 = jnp.float32

    def nrm(k, shape, s):
        return jax.random.normal(k, shape, f32) * s

    def gain(k, shape):
        return 1.0 + 0.05 * jax.random.normal(k, shape, f32)

    return {
        "x": nrm(ks[0], (BATCH, SEQ, D_MODEL), 1.0),
        "c": nrm(ks[1], (BATCH, D_MODEL), 1.0),
        "w_ada": nrm(ks[2], (DEPTH, D_MODEL, 3 * D_MODEL), 0.5 * D_MODEL ** -0.5),
        "b_ada": nrm(ks[3], (DEPTH, 3 * D_MODEL), 0.02),
        "g_pre": gain(ks[4], (DEPTH, D_MODEL)),
        "g_post": gain(ks[5], (DEPTH, D_MODEL)),
        "w_in": nrm(ks[6], (DEPTH, D_MODEL, IN_WIDTH), D_MODEL ** -0.5),
        "g_q_a": gain(ks[7], (DEPTH, MLA_Q_RANK)),
        "w_uq_a": nrm(ks[8], (DEPTH, MLA_Q_RANK, N_HEADS_BRANCH * (MLA_NOPE + MLA_ROPE)), MLA_Q_RANK ** -0.5),
        "g_kv_a": gain(ks[9], (DEPTH, MLA_KV_RANK)),
        "w_ukv_a": nrm(ks[10], (DEPTH, MLA_KV_RANK, N_HEADS_BRANCH * (MLA_NOPE + MLA_V)), MLA_KV_RANK ** -0.5),
        "lam_q1": nrm(ks[11], (DEPTH, DIFF_QK_DIM), 0.1),
        "lam_k1": nrm(ks[12], (DEPTH, DIFF_QK_DIM), 0.1),
        "lam_q2": nrm(ks[13], (DEPTH, DIFF_QK_DIM), 0.1),
        "lam_k2": nrm(ks[14], (DEPTH, DIFF_QK_DIM), 0.1),
        "g_sub_d": gain(ks[15], (DEPTH, HEAD_DIM)),
        "w_out": nrm(ks[16], (DEPTH, MIX_WIDTH, D_MODEL), MIX_WIDTH ** -0.5),
        "rel_bias": nrm(ks[17], (REL_BUCKETS, N_BIAS_HEADS), 0.5),
    }


def reference(x, c, w_ada, b_ada, g_pre, g_post, w_in, g_q_a, w_uq_a, g_kv_a, w_ukv_a,
              lam_q1, lam_k1, lam_q2, lam_k2, g_sub_d, w_out, rel_bias):
    b, s, _ = x.shape
    nh = N_HEADS_BRANCH
    pos = jnp.arange(s)

    def heads(t):
        return t.reshape(b, s, nh, -1)

    for li in range(DEPTH):
        mod = jax.nn.silu(c) @ w_ada[li] + b_ada[li]
        shift, scale, gate = jnp.split(mod, 3, axis=-1)
        h = rmsnorm(x, g_pre[li]) * (1.0 + scale[:, None, :]) + shift[:, None, :]
        p = split_columns(h @ w_in[li])

        q_a = (rmsnorm(p["a_cq"], g_q_a[li]) @ w_uq_a[li]).reshape(b, s, nh, MLA_NOPE + MLA_ROPE)
        kv_a = (rmsnorm(p["a_ckv"], g_kv_a[li]) @ w_ukv_a[li]).reshape(b, s, nh, MLA_NOPE + MLA_V)
        q_a = jnp.concatenate([q_a[..., :MLA_NOPE], apply_rope(q_a[..., MLA_NOPE:], pos)], axis=-1)
        k_rope = apply_rope(p["a_krope"][:, :, None, :], pos)
        k_a = jnp.concatenate([kv_a[..., :MLA_NOPE],
                               jnp.broadcast_to(k_rope, (b, s, nh, MLA_ROPE))], axis=-1)
        out_a = causal_block_attention(q_a, k_a, kv_a[..., MLA_NOPE:], (MLA_NOPE + MLA_ROPE) ** -0.5)

        out_b = dilated_mixture_attention(heads(p["b_q"]), heads(p["b_k"]), heads(p["b_v"]),
                                          rel_bias[:, 0:nh])

        out_c = dsa_attention(heads(p["c_q"]), heads(p["c_k"]), heads(p["c_v"]),
                              p["c_qidx"].reshape(b, s, IDX_HEADS, IDX_DIM), p["c_kidx"], p["c_widx"],
                              rel_bias[:, nh:2 * nh])

        lam_init = 0.8 - 0.6 * math.exp(-0.3 * li)
        lam = (jnp.exp(jnp.sum(lam_q1[li] * lam_k1[li]).astype(jnp.float32))
               - jnp.exp(jnp.sum(lam_q2[li] * lam_k2[li]).astype(jnp.float32)) + lam_init)
        q_d, k_d = heads(p["d_q"]), heads(p["d_k"])
        out_d = diff_attention(q_d[..., :DIFF_QK_DIM], q_d[..., DIFF_QK_DIM:],
                               k_d[..., :DIFF_QK_DIM], k_d[..., DIFF_QK_DIM:],
                               heads(p["d_v"]), lam, rel_bias[:, 2 * nh:3 * nh])
        out_d = rmsnorm(out_d, g_sub_d[li]) * (1.0 - lam_init)

        mixed = jnp.concatenate([o.reshape(b, s, BRANCH_WIDTH) for o in (out_a, out_b, out_c, out_d)],
                                axis=-1) * jax.nn.silu(p["gate"])
        y = mixed @ w_out[li]
        x = x + gate[:, None, :] * rmsnorm(y, g_post[li])
    return x
```

```python
import math
from contextlib import ExitStack

import numpy as np
import ml_dtypes

import concourse.bass as bass
import concourse.mybir as mybir
from concourse.bass_utils import run_bass_kernel_spmd

F32 = mybir.dt.float32
BF16 = mybir.dt.bfloat16
AF = mybir.ActivationFunctionType
ALU = mybir.AluOpType
AX = mybir.AxisListType

D = 2048
S = 4096
NB = 4
DEPTH = 2
R = 2
NSLOT = S // 512 // R
SOWN = S // R
NEG = -30000.0
EPS = 1e-6
OFF = (R - 1) * 512 + 384
WSTRIP = OFF + 2688
CAP_BIAS = OFF + 1664
CAP_A = OFF + 128
OFF2 = 512
W2 = 2176
NBIS = 22

KC_CKV = 0
KC_KRP = 2
KC_KRS = 3
KC_KIDX = 4
KC_BK = 5
KC_CK = 9
KC_DK = 13
NKC = 17
QC_CQ = 0
QC_BQ = 3
QC_CQQ = 7
QC_QIDX = 11
QC_DQ = 27
QC_GATE = 35
NQC = 51


class Buf:
    __slots__ = ("name", "lastw", "readers", "sem", "dmacnt")

    def __init__(self, name):
        self.name = name
        self.lastw = None
        self.readers = {}
        self.sem = None
        self.dmacnt = 0


class Prog:
    def __init__(self, nc, stack):
        self.nc = nc
        self.stack = stack
        self.eng = {"pe": nc.tensor, "act": nc.scalar, "dve": nc.vector, "pool": nc.gpsimd, "sp": nc.sync}
        self.esem = {}
        self.ecnt = {}
        for e in ("pe", "act", "dve", "pool"):
            self.esem[e] = stack.enter_context(nc.semaphore("es_" + e))
            self.ecnt[e] = 0
        self.waited = {e: {} for e in self.eng}
        self.semobj = {}
        self.dmabufs = []
        self.pool = []
        self.nsem = 4

    def buf(self, name):
        return Buf(name)

    def bufs(self, name, n):
        return [Buf(f"{name}{i}") for i in range(n)]

    def _wait(self, e, ev):
        sem, val = ev
        key = id(sem)
        if self.waited[e].get(key, 0) < val:
            self.eng[e].wait_ge(sem, val)
            self.waited[e][key] = val

    def _deps(self, e, reads, writes, strict=False):
        own = None if strict else self.esem.get(e)
        evs = []
        for b in reads:
            if b.lastw is not None:
                evs.append(b.lastw)
        for b in writes:
            if b.lastw is not None:
                evs.append(b.lastw)
            for k, ev in b.readers.items():
                evs.append(ev)
        for ev in evs:
            if own is not None and ev[0] is own:
                continue
            self._wait(e, ev)

    def _commit(self, ev, reads, writes):
        key = id(ev[0])
        for b in writes:
            b.lastw = ev
            b.readers = {}
        for b in reads:
            old = b.readers.get(key)
            if old is None or old[1] < ev[1]:
                b.readers[key] = ev

    def op(self, e, fn, reads=(), writes=(), strict=False):
        self._deps(e, reads, writes, strict)
        ins = fn(self.eng[e])
        self.ecnt[e] += 1
        ins.then_inc(self.esem[e], 1)
        self._commit((self.esem[e], self.ecnt[e]), reads, writes)

    def dma(self, q, sbuf_buf, fns, reads=(), writes=()):
        if sbuf_buf.sem is None:
            if self.pool:
                sbuf_buf.sem = self.pool.pop()
            else:
                h = self.stack.enter_context(self.nc.semaphore("ds_%d" % self.nsem))
                self.nsem += 1
                sbuf_buf.sem = [h, 0]
            self.dmabufs.append(sbuf_buf)
        rec = sbuf_buf.sem
        self._deps(q, reads, writes)
        if rec[1]:
            self._wait(q, (rec[0], rec[1]))
        if not isinstance(fns, (list, tuple)):
            fns = [fns]
        for fn in fns:
            fn(self.eng[q]).then_inc(rec[0], 16)
            rec[1] += 16
        sbuf_buf.dmacnt = rec[1]
        ev = (rec[0], rec[1])
        self._commit(ev, reads, writes)
        return ev

    def barrier(self):
        evs = [(self.esem[e], self.ecnt[e]) for e in self.esem if self.ecnt[e]]
        evs += [(b.sem[0], b.sem[1]) for b in self.dmabufs if b.sem[1]]
        for e in self.eng:
            for ev in evs:
                if self.esem.get(e) is ev[0]:
                    continue
                self._wait(e, ev)
        for b in self.dmabufs:
            self.pool.append(b.sem)
            b.sem = None
        self.dmabufs = []


def _t5_bucket_np(n):
    n = np.asarray(n, dtype=np.int64)
    max_exact = 16
    nf = np.maximum(n, max_exact).astype(np.float32)
    large = max_exact + (np.log(nf / np.float32(max_exact)) / np.float32(math.log(2048 / max_exact))
                         * np.float32(32 - max_exact)).astype(np.int32)
    large = np.minimum(large, 31)
    return np.where(n < max_exact, n, large)


def _chunk_layout(w, cols):
    K = w.shape[0]
    out = np.zeros((len(cols), 128, K // 128, 128), np.float32)
    for i, c in enumerate(cols):
        off = 0
        if isinstance(c, tuple):
            c, off = c
        blk = w[:, c]
        out[i, :, :, off:off + len(c)] = blk.reshape(K // 128, 128, len(c)).transpose(1, 0, 2)
    return out


def _slab_layout(w, cols, width):
    K = w.shape[0]
    out = np.zeros((len(cols), 128, K // 128, width), np.float32)
    for i, c in enumerate(cols):
        blk = w[:, c]
        out[i, :, :, :len(c)] = blk.reshape(K // 128, 128, len(c)).transpose(1, 0, 2)
    return out


def _ar(a, n):
    return np.arange(a, a + n)


def _in_col_offsets():
    names = [("a_cq", 384), ("a_ckv", 256), ("a_krope", 64), ("b_q", 512), ("b_k", 512), ("b_v", 512),
             ("c_q", 512), ("c_k", 512), ("c_v", 512), ("c_qidx", 1024), ("c_kidx", 64), ("c_widx", 16),
             ("d_q", 512), ("d_k", 512), ("d_v", 512), ("gate", 2048)]
    off, o = {}, 0
    for n, w in names:
        off[n] = o
        o += w
    return off


def _rope_swap(c0):
    return np.concatenate([_ar(c0 + 32, 32), _ar(c0, 32)])


def host_weights(w_in, w_uq, w_ukv, w_ada, w_out):
    o = _in_col_offsets()
    kcols = []
    kcols += [_ar(o["a_ckv"] + 128 * i, 128) for i in range(2)]
    kr = _ar(o["a_krope"], 64)
    krs = _rope_swap(o["a_krope"])
    kcols += [np.concatenate([kr, kr]), np.concatenate([krs, krs])]
    ki = _ar(o["c_kidx"], 64)
    kcols += [np.concatenate([ki, ki])]
    for nm in ("b_k", "c_k", "d_k"):
        kcols += [_ar(o[nm] + 128 * i, 128) for i in range(4)]
    assert len(kcols) == NKC
    qcols = []
    qcols += [_ar(o["a_cq"] + 128 * i, 128) for i in range(3)]
    for nm in ("b_q", "c_q"):
        qcols += [_ar(o[nm] + 128 * i, 128) for i in range(4)]
    qcols += [(_ar(o["c_qidx"] + 64 * i, 64), 0) for i in range(16)]
    for h in range(4):
        qcols += [(_ar(o["d_q"] + 128 * h, 64), 0), (_ar(o["d_q"] + 128 * h + 64, 64), 64)]
    qcols += [_ar(o["gate"] + 128 * i, 128) for i in range(16)]
    assert len(qcols) == NQC
    wk = _chunk_layout(w_in, kcols)
    wq = _chunk_layout(w_in, qcols)
    wv = _slab_layout(w_in, [_ar(o[nm], 512) for nm in ("b_v", "c_v", "d_v")], 512)
    wwi = _slab_layout(w_in, [_ar(o["c_widx"], 16)], 16)
    uq = []
    for h in range(4):
        uq.append(_ar(h * 192, 128))
    for pair in range(2):
        uq.append(np.concatenate([_ar((2 * pair + t) * 192 + 128, 64) for t in range(2)]))
    for pair in range(2):
        uq.append(np.concatenate([_rope_swap((2 * pair + t) * 192 + 128) for t in range(2)]))
    wuq = _chunk_layout(w_uq, uq)
    wukn = _chunk_layout(w_ukv, [_ar(h * 256, 128) for h in range(4)])
    wukv = _slab_layout(w_ukv, [np.concatenate([_ar(h * 256 + 128, 128) for h in range(4)])], 512)
    wada = _slab_layout(w_ada, [_ar(512 * i, 512) for i in range(12)], 512)
    wo = _slab_layout(w_out, [_ar(512 * i, 512) for i in range(4)], 512)
    return dict(wk=wk, wq=wq, wv=wv, wwi=wwi, wuq=wuq, wukn=wukn, wukv=wukv, wada=wada, wo=wo)


class Ctx:
    pass


_uid = [0]


def _sb(nc, st, name, shape, dtype):
    _uid[0] += 1
    return st.enter_context(nc.sbuf_tensor("s%d_%s" % (_uid[0], name), list(shape), dtype))


def build_program(layers=(0, 1), debug=None, exchange=False):
    nc = bass.Bass("TRN2", target_bir_lowering=False)
    nl = len(layers)
    dk = "ExternalOutput" if debug else "Internal"
    if debug == "full":
        debug = "__none__"

    def din(name, shape, dt=F32):
        return nc.dram_tensor(name, list(shape), dt, kind="ExternalInput").ap()

    def dscr(name, shape, dt=BF16, kind=None):
        return nc.dram_tensor(name, list(shape), dt, kind=kind or dk).ap()

    specs = dict(
        x_all=[S, D], x_own=[SOWN, D], c_pk=[128, 16], b_ada=[nl, 3 * D], g_pre=[nl, D], g_post=[nl, D],
        wk=[nl, NKC, 128, 16 * 128], wq=[nl, NQC, 128, 16 * 128], wv=[nl, 3, 128, 16 * 512],
        wwi=[nl, 128, 16 * 16], wuq=[nl, 8, 128, 3 * 128], wukn=[nl, 4, 128, 2 * 128], wukv=[nl, 128, 2 * 512],
        wada=[nl, 12, 128, 16 * 512], wo=[nl, 4, 128, 16 * 512], g_q=[nl, 128, 3], g_kv=[nl, 128, 2],
        lam=[nl, 4, 64], g_sub=[nl, 128, 1], ident=[128, 128],
        bstrip=[13, 128, WSTRIP],
        cstrip=[2, 128, WSTRIP],
        strip2=[128, W2],
        ropek=[2, 128, S],
        ropeq=[2, 128, SOWN],
        rel31=[12])

    class LazyIO:
        def __init__(self):
            self.used = []

        def __getattr__(self, name):
            if name in specs:
                ap = din(name, specs[name])
                self.used.append(name)
                setattr(self, name, ap)
                return ap
            raise AttributeError(name)
    io = LazyIO()
    nc.io_used = io.used
    io.out = nc.dram_tensor("out", [SOWN, D], F32, kind="ExternalOutput").ap()

    sc = Ctx()
    sc.kfm = dscr("kfm", [NKC, 128, S])
    sc.qfm = dscr("qfm", [NQC, 128, SOWN])
    sc.vtm = dscr("vtm", [3, 128, 32, 512])
    sc.widx = dscr("widx", [128, SOWN // 128, 16], F32)
    sc.ka = dscr("ka", [5, 128, S])
    sc.va = dscr("va", [128, 32, 512])
    sc.qa = dscr("qa", [6, 128, SOWN])
    sc.strips = dscr("strips", [13, 128, WSTRIP])
    sc.mixed = dscr("mixed", [16, 128, SOWN])
    sc.g2 = dscr("g2", [128, D], F32)
    sc.x1own = dscr("x1own", [SOWN, D], F32)
    sc.x1all = dscr("x1all", [S, D], F32)

    with ExitStack() as st:
        P = Prog(nc, st)
        T = Ctx()
        T.Gb = P.buf("G")
        T.identf = _sb(nc, st, "identf", [128, 128], F32)
        T.ident = _sb(nc, st, "ident", [128, 128], BF16)
        T.ones = _sb(nc, st, "ones", [128, 128], BF16)
        T.epsb = _sb(nc, st, "epsb", [128, 1], F32)
        T.cb = P.buf("consts")
        T.ps = [st.enter_context(nc.psum_tensor("ps%d" % i, [128, 512], F32)) for i in range(8)]
        T.psb = P.bufs("ps", 8)
        T.dram = {}

        def dbuf(name):
            if name not in T.dram:
                T.dram[name] = P.buf("dram_" + name)
            return T.dram[name]
        T.dbuf = dbuf
        T.debug = bool(debug)
        T.ndump = [0]

        def dump(name, ap, shape, dt, reads):
            if not T.debug:
                return
            dst = nc.dram_tensor("dbg_" + name, list(shape), dt, kind="ExternalOutput").ap()
            b = P.buf("dump_" + name)
            P.dma("sp", b, lambda e: e.dma_start(out=dst, in_=ap), reads=reads)
        T.dump = dump

        P.dma("sp", T.cb, lambda e: e.dma_start(out=T.identf[:], in_=io.ident[:, :]), writes=[T.cb])
        P.op("dve", lambda e: e.tensor_copy(out=T.ident[:], in_=T.identf[:]), reads=[T.cb], writes=[T.cb])
        P.op("dve", lambda e: e.memset(T.ones[:], 1.0), writes=[T.cb])
        P.op("dve", lambda e: e.memset(T.epsb[:], EPS), writes=[T.cb])

        phase_strips(nc, P, T, io, sc)
        for lidx, li in enumerate(layers):
            x_all = io.x_all if lidx == 0 else sc.x1all
            x_own = io.x_own if lidx == 0 else sc.x1own
            last = lidx == nl - 1
            x_out = io.out if last else sc.x1own
            stop = False
            with ExitStack() as lst:
                T.G1 = _sb(nc, lst, "G1", [128, D], F32)
                T.SH = _sb(nc, lst, "SH", [128, D], F32)
                phase_mod(nc, P, T, io, sc, lidx)
                if debug == "mod":
                    break
                phase_proj(nc, P, T, io, sc, lidx, x_all, "k", permuted=(lidx > 0))
                if debug == "pk":
                    break
                phase_proj(nc, P, T, io, sc, lidx, x_own, "q")
                if debug == "pq":
                    break
            phase_mla(nc, P, T, io, sc, lidx)
            if debug == "mla":
                break
            for mixer in "ABCD":
                phase_attn(nc, P, T, io, sc, lidx, li, mixer)
                if debug == "attn" + mixer:
                    stop = True
                    break
            if stop or debug == "attn":
                break
            phase_out(nc, P, T, io, sc, lidx, x_own, x_out)
            if not last and exchange:
                phase_exchange(nc, P, T, sc)
        P.barrier()
    return nc


def phase_mod(nc, P, T, io, sc, lidx):
    import os
    CUT = int(os.environ.get("CUT", "99"))
    with ExitStack() as st:
        csb = _sb(nc, st, "m_c", [128, 16], F32)
        scs = _sb(nc, st, "m_sc", [128, 16], F32)
        scb = _sb(nc, st, "m_scb", [128, 16, 128], F32)
        bada = _sb(nc, st, "m_bada", [128, 3 * D], F32)
        gpre = _sb(nc, st, "m_gpre", [128, D], F32)
        gpost = _sb(nc, st, "m_gpost", [128, D], F32)
        modbc = _sb(nc, st, "m_mod", [128, 3 * D], F32)
        stg = [_sb(nc, st, "m_stg%d" % i, [128, 16, 512], F32) for i in range(2)]
        b_c, b_sc, b_bada, b_g, b_mod = P.buf("c"), P.buf("sc"), P.buf("bada"), P.buf("gp"), P.buf("mod")
        b_stg = P.bufs("stg", 2)
        P.dma("sp", b_c, lambda e: e.dma_start(out=csb[:], in_=io.c_pk[:, :]), writes=[b_c])
        P.dma("sp", b_bada, lambda e: e.dma_start(out=bada[:], in_=io.b_ada[lidx, :].partition_broadcast(128)),
              writes=[b_bada])
        P.dma("sp", b_g, [lambda e: e.dma_start(out=gpre[:], in_=io.g_pre[lidx, :].partition_broadcast(128)),
                          lambda e: e.dma_start(out=gpost[:], in_=io.g_post[lidx, :].partition_broadcast(128))],
              writes=[b_g])
        if CUT < 1:
            return
        P.op("act", lambda e: e.activation(out=scs[:], in_=csb[:], func=AF.Silu), reads=[b_c], writes=[b_sc])
        P.op("dve", lambda e: e.tensor_copy(out=scb[:], in_=scs[:].unsqueeze(2).to_broadcast([128, 16, 128])),
             reads=[b_sc], writes=[b_sc])
        if CUT < 2:
            return
        for i in range(12):
            if (CUT == 2 and i > 0) or (CUT == 3 and i > 1) or (CUT == 4 and i > 2):
                return
            s = i % 2
            P.dma("sp", b_stg[s], lambda e: e.dma_start(out=stg[s][:].rearrange("p k c -> p (k c)"),
                                                        in_=io.wada[lidx, i, :, :]), writes=[b_stg[s]])
            pb = T.psb[i % 2]
            ps = T.ps[i % 2]
            for k in range(16):
                P.op("pe", lambda e: e.matmul(ps[:], scb[:, k, :], stg[s][:, k, :], start=(k == 0), stop=(k == 15)),
                     reads=[b_sc, b_stg[s]], writes=[pb])
            P.op("dve", lambda e: e.tensor_tensor(out=modbc[:, i * 512:(i + 1) * 512], in0=ps[:],
                                                  in1=bada[:, i * 512:(i + 1) * 512], op=ALU.add),
                 reads=[pb, b_bada], writes=[b_mod])
        if CUT < 6:
            return
        P.op("dve", lambda e: e.scalar_tensor_tensor(out=T.G1[:], in0=modbc[:, D:2 * D], scalar=1.0, in1=gpre[:],
                                                     op0=ALU.add, op1=ALU.mult), reads=[b_mod, b_g], writes=[T.Gb])
        if CUT == 6:
            return
        P.op("dve", lambda e: e.tensor_copy(out=T.SH[:], in_=modbc[:, 0:D]), reads=[b_mod], writes=[T.Gb])
        if CUT == 7:
            return
        P.op("dve", lambda e: e.tensor_tensor(out=gpre[:], in0=modbc[:, 2 * D:3 * D], in1=gpost[:], op=ALU.mult),
             reads=[b_mod, b_g, T.Gb], writes=[b_g])
        P.dma("pool", b_g, lambda e: e.dma_start(out=sc.g2[:, :], in_=gpre[:]), reads=[b_g], writes=[T.dbuf("g2")])
        P.barrier()


def emit_norm_T(nc, P, T, W, xsrc, xbuf, nblk, permuted=False):
    for tb in range(nblk):
        s = tb % 2
        xt = W.xt[s]
        row = tb * 128
        if permuted:
            qt = tb // 4
            row = (qt % R) * SOWN + (qt // R) * 512 + (tb % 4) * 128
        P.dma("sp", W.b_xt[s], lambda e: e.dma_start(out=xt[:], in_=xsrc[row:row + 128, :]),
              reads=[xbuf], writes=[W.b_xt[s]])
        P.op("act", lambda e: e.activation(out=W.junk[:], in_=xt[:], func=AF.Square, accum_out=W.ss[:, 0:1]),
             reads=[W.b_xt[s]], writes=[W.b_junk, W.b_ss])
        P.op("act", lambda e: e.activation(out=W.ss[:, 1:2], in_=W.ss[:, 0:1], func=AF.Sqrt, bias=T.epsb[:, 0:1],
                                           scale=1.0 / D), reads=[W.b_ss, T.cb], writes=[W.b_ss])
        P.op("dve", lambda e: e.reciprocal(out=W.ss[:, 2:3], in_=W.ss[:, 1:2]), reads=[W.b_ss], writes=[W.b_rs])
        P.op("dve", lambda e: e.scalar_tensor_tensor(out=W.t1[:], in0=xt[:], scalar=W.ss[:, 2:3], in1=T.G1[:],
                                                     op0=ALU.mult, op1=ALU.mult),
             reads=[W.b_xt[s], W.b_rs, T.Gb], writes=[W.b_t1])
        P.op("pool", lambda e: e.tensor_tensor(out=W.hb[:], in0=W.t1[:], in1=T.SH[:], op=ALU.add),
             reads=[W.b_t1, T.Gb], writes=[W.b_hb])
        for half in range(2):
            pb = T.psb[6 + half]
            tp = T.ps[6 + half][:].bitcast(BF16)
            for kk in range(8):
                k = half * 8 + kk
                P.op("pe", lambda e: e.transpose(tp[:, kk * 128:(kk + 1) * 128], W.hb[:, k * 128:(k + 1) * 128],
                                                 T.ident[:]), reads=[W.b_hb, T.cb], writes=[pb])
            eng = "act" if half == 0 else "dve"
            dst = W.hT[:, half * 8:(half + 1) * 8, tb * 128:(tb + 1) * 128]
            src = tp.rearrange("p (k t) -> p k t", k=8)
            if eng == "act":
                P.op("act", lambda e: e.activation(out=dst, in_=src, func=AF.Copy), reads=[pb], writes=[W.b_hT[tb]])
            else:
                P.op("dve", lambda e: e.tensor_copy(out=dst, in_=src), reads=[pb], writes=[W.b_hT[tb]])


def phase_proj(nc, P, T, io, sc, lidx, xsrc, side, permuted=False):
    ntok = S if side == "k" else SOWN
    nblk = ntok // 128
    ntt = ntok // 512
    with ExitStack() as st:
        W = Ctx()
        W.hT = _sb(nc, st, "hT", [128, 16, ntok], BF16)
        W.b_hT = P.bufs("hT", nblk)
        with ExitStack() as st0:
            W.xt = [_sb(nc, st0, "xt%d" % i, [128, D], F32) for i in range(2)]
            W.b_xt = P.bufs("xt", 2)
            W.junk = _sb(nc, st0, "junk", [128, D], BF16)
            W.b_junk = P.buf("junk")
            W.ss = _sb(nc, st0, "ss", [128, 4], F32)
            W.b_ss, W.b_rs = P.buf("ss"), P.buf("rs")
            W.t1 = _sb(nc, st0, "t1", [128, D], F32)
            W.b_t1 = P.buf("t1")
            W.hb = _sb(nc, st0, "hb", [128, D], BF16)
            W.b_hb = P.buf("hb")
            xbuf = T.dbuf("x1all" if side == "k" else "x1own")
            emit_norm_T(nc, P, T, W, xsrc, xbuf, nblk, permuted)
            P.barrier()
        NOB = 4
        ob = [_sb(nc, st, "ob%d" % i, [128, 512], BF16) for i in range(NOB)]
        b_ob = P.bufs("ob", NOB)
        st1 = ExitStack()
        NST = 3
        wst = [_sb(nc, st1, "wst%d" % i, [128, 16, 128], F32) for i in range(NST)]
        b_wst = P.bufs("wst", NST)
        wb = [_sb(nc, st1, "wb%d" % i, [128, 16, 128], BF16) for i in range(2)]
        b_wb = P.bufs("wb", 2)
        if side == "k":
            wsrc, nch, dst = io.wk, NKC, sc.kfm
            spec = {c: (AF.Copy, 1.0) for c in range(NKC)}
            dname = "kfm"
        else:
            wsrc, nch, dst = io.wq, NQC, sc.qfm
            spec = {c: (AF.Copy, 1.0) for c in range(NQC)}
            for c in range(4):
                spec[QC_BQ + c] = (AF.Copy, 128 ** -0.5)
                spec[QC_CQQ + c] = (AF.Copy, 128 ** -0.5)
            for c in range(8):
                spec[QC_DQ + c] = (AF.Copy, 64 ** -0.5)
            for c in range(16):
                spec[QC_GATE + c] = (AF.Silu, 1.0)
            dname = "qfm"

        def load_w(c):
            s = c % NST
            P.dma("sp", b_wst[s], lambda e: e.dma_start(out=wst[s][:].rearrange("p k c -> p (k c)"),
                                                        in_=wsrc[lidx, c, :, :]), writes=[b_wst[s]])

        def cast_w(c):
            s, s2 = c % NST, c % 2
            eng = "pool" if c % 2 == 0 else "dve"
            P.op(eng, lambda e: e.tensor_copy(out=wb[s2][:], in_=wst[s][:]), reads=[b_wst[s]], writes=[b_wb[s2]])

        load_w(0)
        if nch > 1:
            load_w(1)
        cast_w(0)
        oi = 0
        for c in range(nch):
            if c + 2 < nch:
                load_w(c + 2)
            if c + 1 < nch:
                cast_w(c + 1)
            func, scale = spec[c]
            for tt in range(ntt):
                pi = (c * ntt + tt) % 4
                ps, pb = T.ps[pi], T.psb[pi]
                for k in range(16):
                    P.op("pe", lambda e: e.matmul(ps[:], wb[c % 2][:, k, :], W.hT[:, k, tt * 512:(tt + 1) * 512],
                                                  start=(k == 0), stop=(k == 15)),
                         reads=[b_wb[c % 2]] + W.b_hT[tt * 4:(tt + 1) * 4], writes=[pb])
                o = oi % NOB
                oi += 1
                if func == AF.Copy and (oi % 2 == 0):
                    P.op("dve", lambda e: e.tensor_scalar(out=ob[o][:], in0=ps[:], scalar1=float(scale), scalar2=None,
                                                          op0=ALU.mult), reads=[pb], writes=[b_ob[o]])
                else:
                    P.op("act", lambda e: e.activation(out=ob[o][:], in_=ps[:], func=func, scale=float(scale)),
                         reads=[pb], writes=[b_ob[o]])
                P.dma("pool", b_ob[o], lambda e: e.dma_start(out=dst[c, :, tt * 512:(tt + 1) * 512], in_=ob[o][:]),
                      reads=[b_ob[o]], writes=[T.dbuf(dname)])
        P.barrier()
        st1.close()
        if side == "k":
            wv = _sb(nc, st, "wvb", [128, 16, 512], BF16)
            b_wv = P.buf("wv")
            wvs = [_sb(nc, st, "wvs%d" % i, [128, 4, 512], F32) for i in range(2)]
            b_wvs = P.bufs("wvs", 2)
            for m in range(3):
                for q4 in range(4):
                    s = q4 % 2
                    P.dma("sp", b_wvs[s], lambda e: e.dma_start(
                        out=wvs[s][:].rearrange("p k c -> p (k c)"),
                        in_=io.wv[lidx, m, :, q4 * 2048:(q4 + 1) * 2048]), writes=[b_wvs[s]])
                    P.op("dve", lambda e: e.tensor_copy(out=wv[:, q4 * 4:(q4 + 1) * 4, :], in_=wvs[s][:]),
                         reads=[b_wvs[s]], writes=[b_wv])
                for tb in range(nblk):
                    pi = tb % 4
                    ps, pb = T.ps[pi], T.psb[pi]
                    for k in range(16):
                        P.op("pe", lambda e: e.matmul(ps[:], W.hT[:, k, tb * 128:(tb + 1) * 128], wv[:, k, :],
                                                      start=(k == 0), stop=(k == 15)),
                             reads=[b_wv, W.b_hT[tb]], writes=[pb])
                    o = oi % NOB
                    oi += 1
                    if oi % 2 == 0:
                        P.op("dve", lambda e: e.tensor_copy(out=ob[o][:], in_=ps[:]), reads=[pb], writes=[b_ob[o]])
                    else:
                        P.op("act", lambda e: e.activation(out=ob[o][:], in_=ps[:], func=AF.Copy),
                             reads=[pb], writes=[b_ob[o]])
                    P.dma("pool", b_ob[o], lambda e: e.dma_start(out=sc.vtm[m, :, tb, :], in_=ob[o][:]),
                          reads=[b_ob[o]], writes=[T.dbuf("vtm")])
        else:
            wwf = _sb(nc, st, "wwf", [128, 16, 16], F32)
            wwb = _sb(nc, st, "wwb", [128, 16, 16], BF16)
            wio = _sb(nc, st, "wio", [128, nblk, 16], F32)
            b_ww, b_wio = P.buf("ww"), P.buf("wio")
            P.dma("sp", b_ww, lambda e: e.dma_start(out=wwf[:].rearrange("p k c -> p (k c)"), in_=io.wwi[lidx, :, :]),
                  writes=[b_ww])
            P.op("dve", lambda e: e.tensor_copy(out=wwb[:], in_=wwf[:]), reads=[b_ww], writes=[b_ww])
            for tb in range(nblk):
                pi = tb % 4
                ps, pb = T.ps[pi], T.psb[pi]
                for k in range(16):
                    P.op("pe", lambda e: e.matmul(ps[:, 0:16], W.hT[:, k, tb * 128:(tb + 1) * 128], wwb[:, k, :],
                                                  start=(k == 0), stop=(k == 15)),
                         reads=[b_ww, W.b_hT[tb]], writes=[pb])
                P.op("dve", lambda e: e.tensor_copy(out=wio[:, tb, :], in_=ps[:, 0:16]), reads=[pb], writes=[b_wio])
            P.dma("pool", b_wio, lambda e: e.dma_start(out=sc.widx[:, :, :], in_=wio[:]), reads=[b_wio],
                  writes=[T.dbuf("widx")])
        P.barrier()


def host_consts(r):
    i = np.arange(128)[:, None]
    u = np.arange(WSTRIP)[None, :]
    d = u - OFF + 512 * r - i
    causal = np.where(d >= 0, 0.0, NEG).astype(np.float32)
    dd = np.maximum(d, 0)
    m = ((dd <= 128).astype(np.int64) + ((dd % 4 == 0) & (dd <= 512)) + ((dd % 16 == 0) & (dd <= 2048)))
    logm = np.where((d >= 0) & (m > 0), np.log(np.maximum(m, 1)).astype(np.float32), NEG).astype(np.float32)
    cstrip = np.stack([causal, logm]).astype(np.float32)
    bucket = _t5_bucket_np(dd)
    u2 = np.arange(W2)[None, :]
    strip2 = np.where(u2 - i - OFF2 - 512 * r <= 0, 0.0, NEG).astype(np.float32)
    inv = (np.float32(10000.0) ** (-np.arange(32, dtype=np.float32) / np.float32(32))).astype(np.float32)
    p64 = np.arange(128) % 64
    sign = np.where(p64 < 32, -1.0, 1.0).astype(np.float32)[:, None]

    def tables(pos):
        ang = pos.astype(np.float32)[None, :] * inv[p64 % 32][:, None]
        return np.stack([np.cos(ang).astype(np.float32), (sign * np.sin(ang)).astype(np.float32)])
    ropek = tables(np.arange(S))
    own = np.concatenate([np.arange(512) + (R * j + r) * 512 for j in range(NSLOT)])
    ropeq = (tables(own) * np.float32(192 ** -0.5)).astype(np.float32)
    return dict(cstrip=cstrip, bucket=bucket, strip2=strip2, ropek=ropek, ropeq=ropeq)


def host_inputs(inp, layers=(0, 1)):
    f = lambda a: np.ascontiguousarray(np.asarray(a, dtype=np.float32))
    x = f(inp["x"])
    c = f(inp["c"])
    rel_bias = f(inp["rel_bias"])
    per_layer = [host_weights(f(inp["w_in"][li]), f(inp["w_uq_a"][li]), f(inp["w_ukv_a"][li]),
                              f(inp["w_ada"][li]), f(inp["w_out"][li])) for li in layers]
    nl = len(layers)
    shared = {}
    for k, shp in (("wk", (nl, NKC, 128, 2048)), ("wq", (nl, NQC, 128, 2048)), ("wv", (nl, 3, 128, 8192)),
                   ("wwi", (nl, 128, 256)), ("wuq", (nl, 8, 128, 384)), ("wukn", (nl, 4, 128, 256)),
                   ("wukv", (nl, 128, 1024)), ("wada", (nl, 12, 128, 8192)), ("wo", (nl, 4, 128, 8192))):
        shared[k] = np.ascontiguousarray(np.stack([pl[k] for pl in per_layer]).reshape(shp))
    L = list(layers)
    shared["b_ada"] = f(inp["b_ada"])[L]
    shared["g_pre"] = f(inp["g_pre"])[L]
    shared["g_post"] = f(inp["g_post"])[L]
    shared["g_q"] = np.ascontiguousarray(f(inp["g_q_a"])[L].reshape(nl, 3, 128).transpose(0, 2, 1))
    shared["g_kv"] = np.ascontiguousarray(f(inp["g_kv_a"])[L].reshape(nl, 2, 128).transpose(0, 2, 1))
    shared["lam"] = np.ascontiguousarray(np.stack([f(inp[k])[L] for k in ("lam_q1", "lam_k1", "lam_q2", "lam_k2")],
                                                  axis=1))
    shared["g_sub"] = np.ascontiguousarray(f(inp["g_sub_d"])[L].reshape(nl, 128, 1))
    shared["ident"] = np.eye(128, dtype=np.float32)
    consts = [host_consts(r) for r in range(R)]
    bstrips = []
    for r in range(R):
        bk = consts[r]["bucket"]
        bs = np.zeros((13, 128, WSTRIP), np.float32)
        for hh in range(12):
            bs[1 + hh] = rel_bias[bk, hh]
        bstrips.append(bs)
    in_maps = []
    for b in range(NB):
        for r in range(R):
            m = dict(shared)
            m["x_all"] = x[b]
            m["x_own"] = np.ascontiguousarray(x[b].reshape(S // 512, 512, D)[r::R].reshape(SOWN, D))
            m["c_pk"] = np.ascontiguousarray(c[b].reshape(16, 128).T)
            m["bstrip"] = bstrips[r]
            m["cstrip"] = consts[r]["cstrip"]
            m["strip2"] = consts[r]["strip2"]
            m["ropek"] = consts[r]["ropek"]
            m["ropeq"] = consts[r]["ropeq"]
            m["rel31"] = np.ascontiguousarray(rel_bias[31])
            in_maps.append(m)
    return in_maps


def phase_strips(nc, P, T, io, sc):
    with ExitStack() as st:
        cs = _sb(nc, st, "cs", [128, 2, WSTRIP], F32)
        b_cs = P.buf("cs")
        bs = [_sb(nc, st, "bs%d" % i, [128, WSTRIP], F32) for i in range(2)]
        b_bs = P.bufs("bs", 2)
        so = [_sb(nc, st, "so%d" % i, [128, WSTRIP], BF16) for i in range(2)]
        b_so = P.bufs("so", 2)
        P.dma("sp", b_cs, [lambda e: e.dma_start(out=cs[:, 0, :], in_=io.cstrip[0, :, :]),
                           lambda e: e.dma_start(out=cs[:, 1, :], in_=io.cstrip[1, :, :])], writes=[b_cs])
        for idx in range(13):
            s = idx % 2
            kind = 1 if 1 <= idx <= 4 else 0
            P.dma("sp", b_bs[s], lambda e: e.dma_start(out=bs[s][:], in_=io.bstrip[idx, :, :]), writes=[b_bs[s]])
            P.op("dve", lambda e: e.tensor_tensor(out=so[s][:], in0=bs[s][:], in1=cs[:, kind, :], op=ALU.add),
                 reads=[b_bs[s], b_cs], writes=[b_so[s]])
            P.dma("pool", b_so[s], lambda e: e.dma_start(out=sc.strips[idx, :, :], in_=so[s][:]),
                  reads=[b_so[s]], writes=[T.dbuf("strips")])
        P.barrier()


def phase_mla(nc, P, T, io, sc, lidx):
    with ExitStack() as st:
        ckv = _sb(nc, st, "ckv", [128, 2, S], BF16)
        krp = _sb(nc, st, "krp", [128, S], BF16)
        krs = _sb(nc, st, "krs", [128, S], BF16)
        cq = _sb(nc, st, "cq", [128, 3, SOWN], BF16)
        b_in = P.buf("mla_in")
        P.dma("sp", b_in, [lambda e: e.dma_start(out=ckv[:, 0, :], in_=sc.kfm[KC_CKV, :, :]),
                           lambda e: e.dma_start(out=ckv[:, 1, :], in_=sc.kfm[KC_CKV + 1, :, :]),
                           lambda e: e.dma_start(out=krp[:], in_=sc.kfm[KC_KRP, :, :]),
                           lambda e: e.dma_start(out=krs[:], in_=sc.kfm[KC_KRS, :, :])] +
              [(lambda e, i=i: e.dma_start(out=cq[:, i, :], in_=sc.qfm[QC_CQ + i, :, :])) for i in range(3)],
              reads=[T.dbuf("kfm"), T.dbuf("qfm")], writes=[b_in])
        wuqf = _sb(nc, st, "wuqf", [128, 8, 3, 128], F32)
        wuq = _sb(nc, st, "wuq", [128, 8, 3, 128], BF16)
        wknf = _sb(nc, st, "wknf", [128, 4, 2, 128], F32)
        wkn = _sb(nc, st, "wkn", [128, 4, 2, 128], BF16)
        wkvf = _sb(nc, st, "wkvf", [128, 2, 512], F32)
        wkv = _sb(nc, st, "wkv", [128, 2, 512], BF16)
        gq = _sb(nc, st, "gq", [128, 3], F32)
        gkv = _sb(nc, st, "gkv", [128, 2], F32)
        b_w = P.buf("mla_w")
        P.dma("sp", b_w, [(lambda e, c=c: e.dma_start(out=wuqf[:, c, :, :].rearrange("p k c -> p (k c)"),
                                                      in_=io.wuq[lidx, c, :, :])) for c in range(8)] +
              [(lambda e, c=c: e.dma_start(out=wknf[:, c, :, :].rearrange("p k c -> p (k c)"),
                                           in_=io.wukn[lidx, c, :, :])) for c in range(4)] +
              [lambda e: e.dma_start(out=wkvf[:].rearrange("p k c -> p (k c)"), in_=io.wukv[lidx, :, :]),
               lambda e: e.dma_start(out=gq[:], in_=io.g_q[lidx, :, :]),
               lambda e: e.dma_start(out=gkv[:], in_=io.g_kv[lidx, :, :])], writes=[b_w])
        for lc in range(3):
            P.op("dve", lambda e: e.tensor_scalar(out=wuq[:, :, lc, :], in0=wuqf[:, :, lc, :], scalar1=gq[:, lc:lc + 1],
                                                  scalar2=None, op0=ALU.mult), reads=[b_w], writes=[b_w])
        for lc in range(2):
            P.op("dve", lambda e: e.tensor_scalar(out=wkn[:, :, lc, :], in0=wknf[:, :, lc, :], scalar1=gkv[:, lc:lc + 1],
                                                  scalar2=None, op0=ALU.mult), reads=[b_w], writes=[b_w])
            P.op("dve", lambda e: e.tensor_scalar(out=wkv[:, lc, :], in0=wkvf[:, lc, :], scalar1=gkv[:, lc:lc + 1],
                                                  scalar2=None, op0=ALU.mult), reads=[b_w], writes=[b_w])
        sq = _sb(nc, st, "sq", [128, 3, 512], BF16)
        rs = _sb(nc, st, "rsb", [128, 512], F32)
        rsd = _sb(nc, st, "rsd", [128, 512], F32)
        cn = _sb(nc, st, "cn", [128, 3, 512], BF16)
        b_sq, b_rs, b_cn = P.buf("sq"), P.buf("rs"), P.buf("cn")
        rt = [_sb(nc, st, "rt%d" % i, [128, 2, 512], F32) for i in range(2)]
        b_rt = P.bufs("rt", 2)
        t1 = _sb(nc, st, "mt1", [128, 512], F32)
        t2 = _sb(nc, st, "mt2", [128, 512], F32)
        b_t = P.buf("mt")
        NOB = 4
        ob = [_sb(nc, st, "mob%d" % i, [128, 512], BF16) for i in range(NOB)]
        b_ob = P.bufs("mob", NOB)
        cnt = [0]

        def evac_store(ps, pb, dst, dname, mul=None):
            o = cnt[0] % NOB
            cnt[0] += 1
            if mul is None:
                P.op("dve", lambda e: e.tensor_copy(out=ob[o][:], in_=ps[:]), reads=[pb], writes=[b_ob[o]])
            else:
                P.op("dve", lambda e: e.tensor_scalar(out=ob[o][:], in0=ps[:], scalar1=float(mul), scalar2=None,
                                                      op0=ALU.mult), reads=[pb], writes=[b_ob[o]])
            P.dma("pool", b_ob[o], lambda e: e.dma_start(out=dst, in_=ob[o][:]), reads=[b_ob[o]],
                  writes=[T.dbuf(dname)])

        def latent_norm(src, nlc, t0, dim):
            for lc in range(nlc):
                P.op("pool", lambda e: e.tensor_tensor(out=sq[:, lc, :], in0=src[:, lc, t0:t0 + 512],
                                                       in1=src[:, lc, t0:t0 + 512], op=ALU.mult),
                     reads=[b_in], writes=[b_sq])
            ps, pb = T.ps[7], T.psb[7]
            for lc in range(nlc):
                P.op("pe", lambda e: e.matmul(ps[:], T.ones[:], sq[:, lc, :], start=(lc == 0), stop=(lc == nlc - 1)),
                     reads=[b_sq, T.cb], writes=[pb])
            P.op("act", lambda e: e.activation(out=rs[:], in_=ps[:], func=AF.Sqrt, bias=T.epsb[:, 0:1], scale=1.0 / dim),
                 reads=[pb, T.cb], writes=[b_rs])
            P.op("dve", lambda e: e.reciprocal(out=rsd[:], in_=rs[:]), reads=[b_rs], writes=[b_rs])
            for lc in range(nlc):
                P.op("dve", lambda e: e.tensor_tensor(out=cn[:, lc, :], in0=src[:, lc, t0:t0 + 512], in1=rsd[:],
                                                      op=ALU.mult), reads=[b_in, b_rs], writes=[b_cn])

        pi = [0]

        def nextps():
            i = pi[0] % 6
            pi[0] += 1
            return T.ps[i], T.psb[i]

        for tt in range(S // 512):
            t0 = tt * 512
            s = tt % 2
            P.dma("sp", b_rt[s], [lambda e: e.dma_start(out=rt[s][:, 0, :], in_=io.ropek[0, :, t0:t0 + 512]),
                                  lambda e: e.dma_start(out=rt[s][:, 1, :], in_=io.ropek[1, :, t0:t0 + 512])],
                  writes=[b_rt[s]])
            latent_norm(ckv, 2, t0, 256)
            for h in range(4):
                ps, pb = nextps()
                for lc in range(2):
                    P.op("pe", lambda e: e.matmul(ps[:], wkn[:, h, lc, :], cn[:, lc, :], start=(lc == 0), stop=(lc == 1)),
                         reads=[b_w, b_cn], writes=[pb])
                evac_store(ps, pb, sc.ka[h, :, t0:t0 + 512], "ka")
            for q in range(4):
                ps, pb = nextps()
                for lc in range(2):
                    P.op("pe", lambda e: e.matmul(ps[:], cn[:, lc, q * 128:(q + 1) * 128], wkv[:, lc, :],
                                                  start=(lc == 0), stop=(lc == 1)), reads=[b_w, b_cn], writes=[pb])
                evac_store(ps, pb, sc.va[:, tt * 4 + q, :], "va")
            P.op("dve", lambda e: e.tensor_tensor(out=t1[:], in0=krp[:, t0:t0 + 512], in1=rt[s][:, 0, :], op=ALU.mult),
                 reads=[b_in, b_rt[s]], writes=[b_t])
            P.op("pool", lambda e: e.tensor_tensor(out=t2[:], in0=krs[:, t0:t0 + 512], in1=rt[s][:, 1, :], op=ALU.mult),
                 reads=[b_in, b_rt[s]], writes=[b_t])
            o = cnt[0] % NOB
            cnt[0] += 1
            P.op("dve", lambda e: e.tensor_tensor(out=ob[o][:], in0=t1[:], in1=t2[:], op=ALU.add),
                 reads=[b_t], writes=[b_ob[o]])
            P.dma("pool", b_ob[o], lambda e: e.dma_start(out=sc.ka[4, :, t0:t0 + 512], in_=ob[o][:]),
                  reads=[b_ob[o]], writes=[T.dbuf("ka")])
        qscale = 192 ** -0.5
        for tt in range(SOWN // 512):
            t0 = tt * 512
            s = tt % 2
            P.dma("sp", b_rt[s], [lambda e: e.dma_start(out=rt[s][:, 0, :], in_=io.ropeq[0, :, t0:t0 + 512]),
                                  lambda e: e.dma_start(out=rt[s][:, 1, :], in_=io.ropeq[1, :, t0:t0 + 512])],
                  writes=[b_rt[s]])
            latent_norm(cq, 3, t0, 384)
            for h in range(4):
                ps, pb = nextps()
                for lc in range(3):
                    P.op("pe", lambda e: e.matmul(ps[:], wuq[:, h, lc, :], cn[:, lc, :], start=(lc == 0), stop=(lc == 2)),
                         reads=[b_w, b_cn], writes=[pb])
                evac_store(ps, pb, sc.qa[h, :, t0:t0 + 512], "qa", mul=qscale)
            for pair in range(2):
                ps1, pb1 = nextps()
                ps2, pb2 = nextps()
                for lc in range(3):
                    P.op("pe", lambda e: e.matmul(ps1[:], wuq[:, 4 + pair, lc, :], cn[:, lc, :], start=(lc == 0),
                                                  stop=(lc == 2)), reads=[b_w, b_cn], writes=[pb1])
                for lc in range(3):
                    P.op("pe", lambda e: e.matmul(ps2[:], wuq[:, 6 + pair, lc, :], cn[:, lc, :], start=(lc == 0),
                                                  stop=(lc == 2)), reads=[b_w, b_cn], writes=[pb2])
                P.op("dve", lambda e: e.tensor_tensor(out=t1[:], in0=ps1[:], in1=rt[s][:, 0, :], op=ALU.mult),
                     reads=[pb1, b_rt[s]], writes=[b_t])
                P.op("dve", lambda e: e.tensor_tensor(out=t2[:], in0=ps2[:], in1=rt[s][:, 1, :], op=ALU.mult),
                     reads=[pb2, b_rt[s]], writes=[b_t])
                o = cnt[0] % NOB
                cnt[0] += 1
                P.op("dve", lambda e: e.tensor_tensor(out=ob[o][:], in0=t1[:], in1=t2[:], op=ALU.add),
                     reads=[b_t], writes=[b_ob[o]])
                P.dma("pool", b_ob[o], lambda e: e.dma_start(out=sc.qa[4 + pair, :, t0:t0 + 512], in_=ob[o][:]),
                      reads=[b_ob[o]], writes=[T.dbuf("qa")])
        P.barrier()


def kb_hi(j):
    return (R * j + R - 1) * 4 + 4


def phase_attn(nc, P, T, io, sc, lidx, li, mixer):
    mi = "ABCD".index(mixer)
    with ExitStack() as st:
        nk = 5 if mixer == "A" else 4
        nq = {"A": 6, "D": 8}.get(mixer, 4)
        KT = _sb(nc, st, "KT", [128, nk, S], BF16)
        V = _sb(nc, st, "V", [128, 32, 512], BF16)
        QT = _sb(nc, st, "QT", [128, nq, SOWN], BF16)
        nstr = 1 if mixer == "A" else 2
        wS = WSTRIP if mixer == "B" else (CAP_A + 512 if mixer == "A" else CAP_BIAS + 512)
        STR = _sb(nc, st, "STR", [128, nstr, wS], BF16)
        b_K, b_V, b_Q, b_S = P.buf("KT"), P.buf("V"), P.buf("QT"), P.buf("STR")
        if mixer == "A":
            ksrc = [sc.ka[i, :, :] for i in range(5)]
            qsrc = [sc.qa[i, :, :] for i in range(6)]
            vsrc = sc.va
            kdn, qdn, vdn = "ka", "qa", "va"
            sbase = 0
        else:
            kc0 = {"B": KC_BK, "C": KC_CK, "D": KC_DK}[mixer]
            qc0 = {"B": QC_BQ, "C": QC_CQQ, "D": QC_DQ}[mixer]
            ksrc = [sc.kfm[kc0 + i, :, :] for i in range(4)]
            qsrc = [sc.qfm[qc0 + i, :, :] for i in range(nq)]
            vsrc = sc.vtm[mi - 1]
            kdn, qdn, vdn = "kfm", "qfm", "vtm"
            sbase = 1 + 4 * (mi - 1)
        P.dma("sp", b_K, [(lambda e, i=i: e.dma_start(out=KT[:, i, :], in_=ksrc[i])) for i in range(nk)],
              reads=[T.dbuf(kdn)], writes=[b_K])
        P.dma("sp", b_Q, [(lambda e, i=i: e.dma_start(out=QT[:, i, :], in_=qsrc[i])) for i in range(nq)],
              reads=[T.dbuf(qdn)], writes=[b_Q])
        P.dma("sp", b_V, [(lambda e, i=i: e.dma_start(out=V[:, i * 8:(i + 1) * 8, :], in_=vsrc[:, i * 8:(i + 1) * 8, :]))
                          for i in range(4)], reads=[T.dbuf(vdn)], writes=[b_V])
        b_Ss = P.bufs("STRs", 2)
        strip_ctr = [0]

        def load_strip(hd):
            sl = strip_ctr[0] % 2
            strip_ctr[0] += 1
            P.dma("sp", b_Ss[sl], lambda e: e.dma_start(out=STR[:, sl, :], in_=sc.strips[sbase + hd, :, 0:wS]),
                  reads=[T.dbuf("strips")], writes=[b_Ss[sl]])
            return sl
        if mixer == "A":
            load_strip(0)
        Pt = [_sb(nc, st, "Pt%d" % i, [128, 512], BF16) for i in range(3)]
        b_Pt = P.bufs("Pt", 3)
        nmap = 2 if mixer == "D" else 1
        rec = _sb(nc, st, "rec", [128, nmap, 512], F32)
        tq = _sb(nc, st, "tq", [128, nmap, 512], F32)
        b_rec, b_tq = P.buf("rec"), P.buf("tq")
        gt = [_sb(nc, st, "gt%d" % i, [128, 512], BF16) for i in range(2)]
        b_gt = P.bufs("gt", 2)
        mo = [_sb(nc, st, "mo%d" % i, [128, 512], BF16) for i in range(2)]
        b_mo = P.bufs("mo", 2)
        c31 = _sb(nc, st, "c31", [128, 12], F32)
        b_c31 = P.buf("c31")
        P.dma("sp", b_c31, lambda e: e.dma_start(out=c31[:], in_=io.rel31[:].partition_broadcast(128)), writes=[b_c31])
        if mixer == "D":
            lamt = _sb(nc, st, "lamt", [128, 4, 64], F32)
            lw = _sb(nc, st, "lw", [128, 2, 64], F32)
            ls = _sb(nc, st, "ls", [128, 8], F32)
            gsub = _sb(nc, st, "gsub", [128, 2], F32)
            b_lam = P.buf("lam")
            lam_init = 0.8 - 0.6 * math.exp(-0.3 * li)
            P.dma("sp", b_lam, [lambda e: e.dma_start(out=lamt[:].rearrange("p a b -> p (a b)"),
                                                      in_=io.lam[lidx, :, :].rearrange("a b -> (a b)").partition_broadcast(128)),
                                lambda e: e.dma_start(out=gsub[:, 0:1], in_=io.g_sub[lidx, :, :])], writes=[b_lam])
            for m in range(2):
                P.op("dve", lambda e: e.tensor_tensor(out=lw[:, m, :], in0=lamt[:, 2 * m, :], in1=lamt[:, 2 * m + 1, :],
                                                      op=ALU.mult), reads=[b_lam], writes=[b_lam], strict=True)
                P.op("dve", lambda e: e.reduce_sum(out=ls[:, m:m + 1], in_=lw[:, m, :], axis=AX.X),
                     reads=[b_lam], writes=[b_lam], strict=True)
            P.op("act", lambda e: e.activation(out=ls[:, 2:4], in_=ls[:, 0:2], func=AF.Exp), reads=[b_lam], writes=[b_lam], strict=True)
            P.op("dve", lambda e: e.tensor_tensor(out=ls[:, 4:5], in0=ls[:, 3:4], in1=ls[:, 2:3], op=ALU.subtract),
                 reads=[b_lam], writes=[b_lam], strict=True)
            P.op("dve", lambda e: e.tensor_scalar(out=ls[:, 4:5], in0=ls[:, 4:5], scalar1=-lam_init, scalar2=None,
                                                  op0=ALU.add), reads=[b_lam], writes=[b_lam], strict=True)
            P.op("dve", lambda e: e.tensor_scalar(out=gsub[:, 1:2], in0=gsub[:, 0:1], scalar1=1.0 - lam_init,
                                                  scalar2=None, op0=ALU.mult), reads=[b_lam], writes=[b_lam], strict=True)
            dd = _sb(nc, st, "dd", [128, 512], F32)
            dsq = _sb(nc, st, "dsq", [128, 512], BF16)
            drs = _sb(nc, st, "drs", [128, 512], F32)
            b_dd = P.buf("dd")
        if mixer == "C":
            IX = Ctx()
            IX.sc = sc
            IX.qidx = _sb(nc, st, "qidx", [128, 16, 512], BF16)
            IX.b_qidx = P.buf("qidx")
            IX.kidx = _sb(nc, st, "kidx", [128, S], BF16)
            IX.widx = _sb(nc, st, "widxs", [128, SOWN // 128, 16], F32)
            IX.s2 = _sb(nc, st, "s2", [128, W2], BF16)
            IX.score = _sb(nc, st, "score", [128, S], F32)
            IX.b_score = P.buf("score")
            IX.b_in = P.buf("ix_in")
            P.dma("sp", IX.b_in,
                  [lambda e: e.dma_start(out=IX.kidx[:], in_=sc.kfm[KC_KIDX, :, :]),
                   lambda e: e.dma_start(out=IX.widx[:], in_=sc.widx[:, :, :])],
                  reads=[T.dbuf("kfm"), T.dbuf("widx")], writes=[IX.b_in])
            P.dma("sp", IX.b_score, lambda e: e.dma_start(out=IX.score[:, 0:W2], in_=io.strip2[:, :]), writes=[IX.b_score])
            P.op("dve", lambda e: e.tensor_copy(out=IX.s2[:], in_=IX.score[:, 0:W2]), reads=[IX.b_score], writes=[IX.b_in])
            IX.selT = _sb(nc, st, "selT", [128, 32, 512], BF16)
            IX.b_selT = P.buf("selT")
            IX.selm = _sb(nc, st, "selm", [128, S], BF16)
            IX.b_selm = P.buf("selm")
            IX.diag = _sb(nc, st, "diag", [128, 16, 128], BF16)
            IX.b_diag = P.buf("diag")
            IX.wsc = _sb(nc, st, "wsc", [128, 16], F32)
            IX.rl = [_sb(nc, st, "rl%d" % i, [128, 512], BF16) for i in range(3)]
            IX.b_rl = P.bufs("rl", 3)
            IX.bis = _sb(nc, st, "bis", [128, 8], F32)
            IX.b_bis = P.buf("bis")

        def kq_parts(h, m):
            if mixer == "A":
                lo = 64 * (h % 2)
                return [(lambda kb: KT[:, h, kb * 128:(kb + 1) * 128], lambda j: QT[:, h, j * 512:(j + 1) * 512]),
                        (lambda kb: KT[lo:lo + 64, 4, kb * 128:(kb + 1) * 128],
                         lambda j: QT[lo:lo + 64, 4 + h // 2, j * 512:(j + 1) * 512])]
            if mixer == "D":
                return [(lambda kb: KT[:, h, kb * 128:(kb + 1) * 128],
                         lambda j: QT[:, 2 * h + m, j * 512:(j + 1) * 512])]
            return [(lambda kb: KT[:, h, kb * 128:(kb + 1) * 128], lambda j: QT[:, h, j * 512:(j + 1) * 512])]

        nmap = 2 if mixer == "D" else 1
        seti = [0]
        gi = [0]
        for j in range(NSLOT):
            if mixer == "C":
                emit_indexer(nc, P, T, IX, j)
            kbs = []
            for kb in range(kb_hi(j)):
                base_s = R * j * 512 - kb * 128
                if mixer == "B" and base_s >= 2304:
                    continue
                kbs.append((kb, base_s))
            n = len(kbs)
            if mixer == "C" and j == 0:
                T.dump("c_selT", IX.selT[:, 0:8, :].rearrange("p a b -> p (a b)"), [128, 8 * 512], BF16, [IX.b_selT])
            for h in range(4):
                sets = []
                if mixer == "A":
                    ssl = 0
                else:
                    if j == 0 and h == 0:
                        nxt_sl = load_strip(0)
                    ssl = nxt_sl
                    if not (j == NSLOT - 1 and h == 3):
                        nxt_sl = load_strip((h + 1) % 4)
                for m in range(nmap):
                    if mixer == "D":
                        so = 2 + 2 * m
                    else:
                        so = 2 + 2 * (seti[0] % 2)
                        seti[0] += 1
                    sets.append(so)
                    parts = kq_parts(h, m)
                    hh = (mi - 1) * 4 + h

                    def emit_qk(idx):
                        kb, base_s = kbs[idx]
                        ps, pb = T.ps[idx % 2], T.psb[idx % 2]
                        col0 = base_s + OFF
                        need_strip, far = True, False
                        if mixer == "A":
                            if base_s >= 128:
                                need_strip = False
                        elif mixer != "B":
                            if col0 >= CAP_BIAS:
                                need_strip, far = False, True
                        extra = (1 if need_strip else 0) + (1 if mixer == "C" else 0)
                        nmm = len(parts) + extra
                        k = 0
                        for (kf, qf) in parts:
                            P.op("pe", lambda e: e.matmul(ps[:], kf(kb), qf(j), start=(k == 0), stop=(k == nmm - 1)),
                                 reads=[b_K, b_Q], writes=[pb])
                            k += 1
                        if need_strip:
                            P.op("pe", lambda e: e.matmul(ps[:], T.ident[:], STR[:, ssl, col0:col0 + 512],
                                                          start=False, stop=(k == nmm - 1)),
                                 reads=[b_Ss[ssl], T.cb], writes=[pb])
                            k += 1
                        if mixer == "C":
                            P.op("pe", lambda e: e.matmul(ps[:], T.ident[:], IX.selT[:, kb, :], start=False, stop=True),
                                 reads=[IX.b_selT, T.cb], writes=[pb])
                        return far

                    def emit_rest(idx, far):
                        kb, base_s = kbs[idx]
                        ps, pb = T.ps[idx % 2], T.psb[idx % 2]
                        pt, bpt = Pt[idx % 3], b_Pt[idx % 3]
                        if far:
                            P.op("act", lambda e: e.activation(out=pt[:], in_=ps[:], func=AF.Exp, bias=c31[:, hh:hh + 1]),
                                 reads=[pb, b_c31], writes=[bpt])
                        else:
                            P.op("act", lambda e: e.activation(out=pt[:], in_=ps[:], func=AF.Exp), reads=[pb], writes=[bpt])
                        if mixer == "D" and j == 0 and h == 0 and idx == 0:
                            T.dump("d_pt%d" % m, pt[:], [128, 512], BF16, [bpt])
                        P.op("pe", lambda e: e.matmul(T.ps[so][:], V[:, kb, h * 128:(h + 1) * 128], pt[:],
                                                      start=(idx == 0), stop=(idx == n - 1)),
                             reads=[b_V, bpt], writes=[T.psb[so]])
                        P.op("pe", lambda e: e.matmul(T.ps[so + 1][:], T.ones[:], pt[:], start=(idx == 0), stop=(idx == n - 1)),
                             reads=[bpt, T.cb], writes=[T.psb[so + 1]])

                    fars = {0: emit_qk(0)}
                    for idx in range(n):
                        if idx + 1 < n:
                            fars[idx + 1] = emit_qk(idx + 1)
                        emit_rest(idx, fars[idx])
                g = gi[0] % 2
                gi[0] += 1
                chunk = mi * 4 + h
                P.dma("sp", b_gt[g], lambda e: e.dma_start(out=gt[g][:], in_=sc.qfm[QC_GATE + chunk, :, j * 512:(j + 1) * 512]),
                      reads=[T.dbuf("qfm")], writes=[b_gt[g]])
                for m, so in enumerate(sets):
                    P.op("dve", lambda e: e.reciprocal(out=rec[:, m, :], in_=T.ps[so + 1][:]), reads=[T.psb[so + 1]],
                         writes=[b_rec])
                    P.op("dve", lambda e: e.tensor_tensor(out=tq[:, m, :], in0=T.ps[so][:], in1=rec[:, m, :], op=ALU.mult),
                         reads=[T.psb[so], b_rec], writes=[b_tq])
                if mixer == "D" and j == 0 and h == 0:
                    T.dump("d_tq", tq[:].rearrange("p a b -> p (a b)"), [128, 1024], F32, [b_tq])
                    T.dump("d_rec", rec[:].rearrange("p a b -> p (a b)"), [128, 1024], F32, [b_rec])
                    T.dump("d_ls", ls[:], [128, 8], F32, [b_lam])
                if mixer != "D":
                    P.op("dve", lambda e: e.tensor_tensor(out=mo[g][:], in0=tq[:, 0, :], in1=gt[g][:], op=ALU.mult),
                         reads=[b_tq, b_gt[g]], writes=[b_mo[g]])
                else:
                    P.op("dve", lambda e: e.scalar_tensor_tensor(out=dd[:], in0=tq[:, 1, :], scalar=ls[:, 4:5],
                                                                 in1=tq[:, 0, :], op0=ALU.mult, op1=ALU.add),
                         reads=[b_tq, b_lam], writes=[b_dd])
                    P.op("pool", lambda e: e.tensor_tensor(out=dsq[:], in0=dd[:], in1=dd[:], op=ALU.mult),
                         reads=[b_dd], writes=[b_dd])
                    P.op("pe", lambda e: e.matmul(T.ps[6][:], T.ones[:], dsq[:], start=True, stop=True),
                         reads=[b_dd, T.cb], writes=[T.psb[6]])
                    P.op("act", lambda e: e.activation(out=drs[:], in_=T.ps[6][:], func=AF.Sqrt, bias=T.epsb[:, 0:1],
                                                       scale=1.0 / 128), reads=[T.psb[6], T.cb], writes=[b_dd])
                    P.op("dve", lambda e: e.reciprocal(out=drs[:], in_=drs[:]), reads=[b_dd], writes=[b_dd])
                    P.op("dve", lambda e: e.scalar_tensor_tensor(out=dd[:], in0=dd[:], scalar=gsub[:, 1:2], in1=drs[:],
                                                                 op0=ALU.mult, op1=ALU.mult),
                         reads=[b_dd, b_lam], writes=[b_dd])
                    P.op("dve", lambda e: e.tensor_tensor(out=mo[g][:], in0=dd[:], in1=gt[g][:], op=ALU.mult),
                         reads=[b_dd, b_gt[g]], writes=[b_mo[g]])
                P.dma("pool", b_mo[g], lambda e: e.dma_start(out=sc.mixed[chunk, :, j * 512:(j + 1) * 512], in_=mo[g][:]),
                      reads=[b_mo[g]], writes=[T.dbuf("mixed")])
        P.barrier()


def emit_indexer(nc, P, T, IX, j):
    nkc = R * j + R
    L = nkc * 512
    P.dma("sp", IX.b_qidx, [(lambda e, i=i: e.dma_start(out=IX.qidx[:, i, :],
                                                        in_=IX.sc.qfm[QC_QIDX + i, :, j * 512:(j + 1) * 512]))
                            for i in range(16)], reads=[T.dbuf("qfm")], writes=[IX.b_qidx])
    for s in range(4):
        blk = j * 4 + s
        t0 = blk * 128
        P.op("pool", lambda e: e.tensor_scalar(out=IX.wsc[:], in0=IX.widx[:, blk, :], scalar1=1.0 / 32, scalar2=None,
                                               op0=ALU.mult), reads=[IX.b_in], writes=[IX.b_diag])
        for ih in range(16):
            P.op("pool", lambda e: e.tensor_scalar(out=IX.diag[:, ih, :], in0=T.ident[:], scalar1=IX.wsc[:, ih:ih + 1],
                                                   scalar2=None, op0=ALU.mult), reads=[T.cb, IX.b_diag],
                 writes=[IX.b_diag], strict=True)
        for kc in range(nkc):
            base2 = R * j * 512 + s * 128 - kc * 512
            need_mask = base2 < 511
            sp_, sb_ = T.ps[2 + 2 * (kc % 2)], T.psb[2 + 2 * (kc % 2)]

            def dots(ih):
                dp, db = T.ps[6 + ih % 2], T.psb[6 + ih % 2]
                P.op("pe", lambda e: e.matmul(dp[:], IX.qidx[:, ih, s * 128:(s + 1) * 128],
                                              IX.kidx[:, kc * 512:(kc + 1) * 512], start=True, stop=True),
                     reads=[IX.b_in, IX.b_qidx], writes=[db])

            dots(0)
            for ih in range(16):
                if ih + 1 < 16:
                    dots(ih + 1)
                dp, db = T.ps[6 + ih % 2], T.psb[6 + ih % 2]
                rl, brl = IX.rl[ih % 3], IX.b_rl[ih % 3]
                P.op("act", lambda e: e.activation(out=rl[:], in_=dp[:], func=AF.Relu), reads=[db], writes=[brl])
                P.op("pe", lambda e: e.matmul(sp_[:], IX.diag[:, ih, :], rl[:], start=(ih == 0),
                                              stop=(ih == 15 and not need_mask)), reads=[IX.b_diag, brl], writes=[sb_])
            if need_mask:
                col0 = OFF2 - base2
                P.op("pe", lambda e: e.matmul(sp_[:], T.ident[:], IX.s2[:, col0:col0 + 512], start=False, stop=True),
                     reads=[IX.b_in, T.cb], writes=[sb_])
            P.op("dve", lambda e: e.tensor_copy(out=IX.score[:, kc * 512:(kc + 1) * 512], in_=sp_[:]),
                 reads=[sb_], writes=[IX.b_score])
        bis = IX.bis
        P.op("dve", lambda e: e.reduce_max(out=bis[:, 0:1], in_=IX.score[:, 0:L], axis=AX.X), reads=[IX.b_score],
             writes=[IX.b_bis], strict=True)
        LO0 = -64.0
        P.op("dve", lambda e: e.tensor_scalar(out=bis[:, 1:2], in0=bis[:, 0:1], scalar1=-LO0, scalar2=None, op0=ALU.add),
             reads=[IX.b_bis], writes=[IX.b_bis], strict=True)
        P.op("dve", lambda e: e.memset(bis[:, 2:3], LO0), writes=[IX.b_bis], strict=True)
        for it in range(1, NBIS + 1):
            ci = 2.0 ** -it
            P.op("dve", lambda e: e.scalar_tensor_tensor(out=bis[:, 3:4], in0=bis[:, 1:2], scalar=ci, in1=bis[:, 2:3],
                                                         op0=ALU.mult, op1=ALU.add), reads=[IX.b_bis], writes=[IX.b_bis], strict=True)
            P.op("dve", lambda e: e.tensor_scalar(out=IX.selm[:, 0:L], in0=IX.score[:, 0:L], scalar1=bis[:, 3:4],
                                                  scalar2=0.0, op0=ALU.is_ge, op1=ALU.add, accum_out=bis[:, 4:5]),
                 reads=[IX.b_score, IX.b_bis], writes=[IX.b_selm, IX.b_bis], strict=True)
            P.op("dve", lambda e: e.tensor_scalar(out=bis[:, 5:6], in0=bis[:, 4:5], scalar1=256.0, scalar2=ci,
                                                  op0=ALU.is_ge, op1=ALU.mult), reads=[IX.b_bis], writes=[IX.b_bis], strict=True)
            P.op("dve", lambda e: e.scalar_tensor_tensor(out=bis[:, 2:3], in0=bis[:, 5:6], scalar=bis[:, 1:2],
                                                         in1=bis[:, 2:3], op0=ALU.mult, op1=ALU.add),
                 reads=[IX.b_bis], writes=[IX.b_bis], strict=True)
        P.op("dve", lambda e: e.tensor_scalar(out=IX.selm[:, 0:L], in0=IX.score[:, 0:L], scalar1=bis[:, 2:3], scalar2=NEG,
                                              op0=ALU.is_lt, op1=ALU.mult), reads=[IX.b_score, IX.b_bis],
             writes=[IX.b_selm], strict=True)
        if (j, s) in ((0, 0), (1, 1)):
            T.dump("c_score%d" % j, IX.score[:, 0:L], [128, L], F32, [IX.b_score])
            T.dump("c_selm%d" % j, IX.selm[:, 0:L], [128, L], BF16, [IX.b_selm])
            T.dump("c_bis%d" % j, IX.bis[:], [128, 8], F32, [IX.b_bis])
        nkb = L // 128
        for g0 in range(0, nkb, 8):
            tb_i = 3 + 2 * ((g0 // 8) % 2)
            tp = T.ps[tb_i][:].bitcast(BF16)
            for kk in range(8):
                kb = g0 + kk
                P.op("pe", lambda e: e.transpose(tp[:, kk * 128:(kk + 1) * 128], IX.selm[:, kb * 128:(kb + 1) * 128],
                                                 T.ident[:]), reads=[IX.b_selm, T.cb], writes=[T.psb[tb_i]])
            P.op("dve", lambda e: e.tensor_copy(out=IX.selT[:, g0:g0 + 8, s * 128:(s + 1) * 128],
                                                in_=tp.rearrange("p (k t) -> p k t", k=8)),
                 reads=[T.psb[tb_i]], writes=[IX.b_selT])


def phase_out(nc, P, T, io, sc, lidx, x_own, x_out):
    with ExitStack() as st:
        mixT = _sb(nc, st, "mixT", [128, 16, SOWN], BF16)
        wob = _sb(nc, st, "wob", [128, 16, D], BF16)
        G2 = _sb(nc, st, "G2o", [128, D], F32)
        wos = [_sb(nc, st, "wos%d" % i, [128, 4, 512], F32) for i in range(2)]
        xo = [_sb(nc, st, "xo%d" % i, [128, D], F32) for i in range(2)]
        xn = [_sb(nc, st, "xn%d" % i, [128, D], F32) for i in range(2)]
        tt = _sb(nc, st, "ott", [128, D], F32)
        junk = _sb(nc, st, "ojunk", [128, 512], BF16)
        ssq = _sb(nc, st, "ossq", [128, 8], F32)
        b_mix, b_wob, b_G2, b_tt, b_junk, b_ssq = (P.buf("mixT"), P.buf("wob"), P.buf("G2o"), P.buf("ott"),
                                                    P.buf("ojunk"), P.buf("ossq"))
        b_wos, b_xo, b_xn = P.bufs("wos", 2), P.bufs("xo", 2), P.bufs("xn", 2)
        P.dma("sp", b_mix, [(lambda e, c=c: e.dma_start(out=mixT[:, c, :], in_=sc.mixed[c, :, :])) for c in range(16)],
              reads=[T.dbuf("mixed")], writes=[b_mix])
        P.dma("sp", b_G2, lambda e: e.dma_start(out=G2[:], in_=sc.g2[:, :]), reads=[T.dbuf("g2")], writes=[b_G2])
        i = 0
        for m in range(4):
            for q4 in range(4):
                s = i % 2
                P.dma("sp", b_wos[s], lambda e: e.dma_start(out=wos[s][:].rearrange("p k c -> p (k c)"),
                                                            in_=io.wo[lidx, m, :, q4 * 2048:(q4 + 1) * 2048]),
                      writes=[b_wos[s]])
                eng = "dve" if i % 2 == 0 else "pool"
                P.op(eng, lambda e: e.tensor_copy(out=wob[:, q4 * 4:(q4 + 1) * 4, m * 512:(m + 1) * 512], in_=wos[s][:]),
                     reads=[b_wos[s]], writes=[b_wob])
                i += 1
        xbuf = T.dbuf("x1own")
        for tb in range(SOWN // 128):
            s = tb % 2
            bs = (tb % 2) * 4
            P.dma("sp", b_xo[s], lambda e: e.dma_start(out=xo[s][:], in_=x_own[tb * 128:(tb + 1) * 128, :]),
                  reads=[xbuf], writes=[b_xo[s]])
            for m in range(4):
                ps, pb = T.ps[bs + m], T.psb[bs + m]
                for c in range(16):
                    P.op("pe", lambda e: e.matmul(ps[:], mixT[:, c, tb * 128:(tb + 1) * 128], wob[:, c, m * 512:(m + 1) * 512],
                                                  start=(c == 0), stop=(c == 15)), reads=[b_mix, b_wob], writes=[pb])
            for m in range(4):
                P.op("act", lambda e: e.activation(out=junk[:], in_=T.ps[bs + m][:], func=AF.Square,
                                                   accum_out=ssq[:, m:m + 1]), reads=[T.psb[bs + m]],
                     writes=[b_junk, b_ssq])
            P.op("dve", lambda e: e.reduce_sum(out=ssq[:, 4:5], in_=ssq[:, 0:4], axis=AX.X), reads=[b_ssq], writes=[b_ssq])
            P.op("act", lambda e: e.activation(out=ssq[:, 5:6], in_=ssq[:, 4:5], func=AF.Sqrt, bias=T.epsb[:, 0:1],
                                               scale=1.0 / D), reads=[b_ssq, T.cb], writes=[b_ssq])
            P.op("dve", lambda e: e.reciprocal(out=ssq[:, 6:7], in_=ssq[:, 5:6]), reads=[b_ssq], writes=[b_ssq])
            for m in range(4):
                P.op("dve", lambda e: e.tensor_tensor(out=tt[:, m * 512:(m + 1) * 512], in0=T.ps[bs + m][:],
                                                      in1=G2[:, m * 512:(m + 1) * 512], op=ALU.mult),
                     reads=[T.psb[bs + m], b_G2], writes=[b_tt])
            P.op("dve", lambda e: e.scalar_tensor_tensor(out=xn[s][:], in0=tt[:], scalar=ssq[:, 6:7], in1=xo[s][:],
                                                         op0=ALU.mult, op1=ALU.add),
                 reads=[b_tt, b_ssq, b_xo[s]], writes=[b_xn[s]], strict=True)
            P.dma("pool", b_xn[s], lambda e: e.dma_start(out=x_out[tb * 128:(tb + 1) * 128, :], in_=xn[s][:]),
                  reads=[b_xn[s]], writes=[T.dbuf("x1own")])
        P.barrier()


def phase_exchange(nc, P, T, sc):
    groups = [[2 * b, 2 * b + 1] for b in range(NB)]
    xb = T.dbuf("x1own")
    ab = T.dbuf("x1all")
    if not hasattr(T, "ccsem"):
        T.ccbuf = P.buf("cc")
    P.barrier()
    P.dma("pool", T.ccbuf, lambda e: e.collective_compute("AllGather", ALU.bypass, replica_groups=groups,
                                                         ins=[sc.x1own[:, :]], outs=[sc.x1all[:, :]]),
          reads=[xb], writes=[ab])
    P.barrier()


def _assemble(results):
    out = np.empty((NB, S, D), np.float32)
    for b in range(NB):
        v = out[b].reshape(S // 512, 512, D)
        for r in range(R):
            v[r::R] = np.asarray(results[b * R + r]["out"]).reshape(NSLOT, 512, D)
    return out


FUSED = False


def kernel(**inputs):
    inputs = {k: np.asarray(v) for k, v in inputs.items()}
    if FUSED:
        in_maps = host_inputs(inputs, layers=(0, 1))
        nc = build_program(layers=(0, 1), exchange=True)
        in_maps = [{k: m[k] for k in nc.io_used} for m in in_maps]
        res = run_bass_kernel_spmd(nc, in_maps, core_ids=list(range(NB * R)))
        return _assemble(res.results)
    x = inputs["x"]
    for li in range(DEPTH):
        inp = dict(inputs)
        inp["x"] = x
        in_maps = host_inputs(inp, layers=(li,))
        nc = build_program(layers=(li,))
        in_maps = [{k: m[k] for k in nc.io_used} for m in in_maps]
        res = run_bass_kernel_spmd(nc, in_maps, core_ids=list(range(NB * R)))
        x = _assemble(res.results)
    return x
```
